# Optimizing a Trainium2 kernel written in Bass

```python
import jax
import jax.numpy as jnp
from jax import lax
import numpy as np

D_MODEL = 1024
BATCH = 2
SEQ = 8192
DEPTH = 2

GRID_W = 64
CTX_LEN = 256
MLA_HEADS = 8
QK_NOPE = 128
QK_ROPE = 64
QK_DIM = QK_NOPE + QK_ROPE
V_HEAD = 128
Q_LORA = 256
KV_LORA = 128
ROPE_THETA = 10000.0
ATTN_SCALE = QK_DIM ** -0.5
Q_BLOCK = 128
LRU_WIDTH = D_MODEL
LRU_BLOCKS = 16
LRU_BW = LRU_WIDTH // LRU_BLOCKS
CONV_W = 4
LRU_C = 8.0
N_EXPERTS = 32
TOP_K = 4
D_FF = D_MODEL
SWIGLU_ALPHA = 1.702
SWIGLU_LIMIT = 7.0
MOE_BLOCK = 128
NORM_EPS = 1e-6
IN_SPLITS = (Q_LORA, KV_LORA, QK_ROPE, LRU_WIDTH, LRU_WIDTH, D_MODEL, D_MODEL)
D_IN = Q_LORA + KV_LORA + QK_ROPE + 2 * LRU_WIDTH + 2 * D_MODEL

kernel_name = 'hybrid_mla_rglru_moe_dit'


def rmsnorm(x, g):
    xf = x.astype(jnp.float32)
    y = xf * lax.rsqrt(jnp.mean(xf * xf, axis=-1, keepdims=True) + NORM_EPS)
    return (y * g.astype(jnp.float32)).astype(x.dtype)


def modulate(x, shift, scale):
    return x * (1 + scale) + shift


def adaln(cond, w, b):
    m = jax.nn.silu(cond) @ w + b
    return jnp.split(m[:, None, :], 6, axis=-1)


def split_cols(z):
    offs = []
    acc = 0
    for wdt in IN_SPLITS[:-1]:
        acc += wdt
        offs.append(acc)
    return jnp.split(z, offs, axis=-1)


def axial_rope_tables(n_tokens):
    rows = n_tokens // GRID_W
    row = jnp.repeat(jnp.arange(rows, dtype=jnp.float32), GRID_W)
    col = jnp.tile(jnp.arange(GRID_W, dtype=jnp.float32), rows)
    n_freq = QK_ROPE // 4
    inv = ROPE_THETA ** (-jnp.arange(n_freq, dtype=jnp.float32) / n_freq)
    ang = jnp.concatenate([row[:, None] * inv, col[:, None] * inv], axis=-1)
    return jnp.cos(ang), jnp.sin(ang)


def apply_axial_rope(x, cos, sin):
    xr = x.reshape(x.shape[:-1] + (2, 2, QK_ROPE // 4))
    x1, x2 = xr[..., 0, :], xr[..., 1, :]
    c = cos.reshape(cos.shape[:-1] + (2, QK_ROPE // 4))
    s = sin.reshape(sin.shape[:-1] + (2, QK_ROPE // 4))
    out = jnp.stack([x1 * c - x2 * s, x1 * s + x2 * c], axis=-2)
    return out.reshape(x.shape)


def mla_queries(c_q, p, cos, sin):
    b, n, _ = c_q.shape
    q = (rmsnorm(c_q, p['q_norm']) @ p['w_uq']).reshape(b, n, MLA_HEADS, QK_DIM)
    if cos is None:
        return q
    q_pe = apply_axial_rope(q[..., QK_NOPE:], cos[:, None, :], sin[:, None, :])
    return jnp.concatenate([q[..., :QK_NOPE], q_pe], axis=-1)


def mla_keys_values(c_kv, k_pe, p, cos, sin):
    b, n, _ = c_kv.shape
    kv = (rmsnorm(c_kv, p['kv_norm']) @ p['w_ukv']).reshape(b, n, MLA_HEADS, QK_NOPE + V_HEAD)
    k_nope, v = kv[..., :QK_NOPE], kv[..., QK_NOPE:]
    if cos is not None:
        k_pe = apply_axial_rope(k_pe, cos, sin)
    k_pe = jnp.broadcast_to(k_pe[:, :, None, :], (b, n, MLA_HEADS, QK_ROPE))
    return jnp.concatenate([k_nope, k_pe], axis=-1), v


def softmax_attend(q, k, v):
    s = jnp.einsum('bqhd,bkhd->bhqk', q, k).astype(jnp.float32) * ATTN_SCALE
    pr = jax.nn.softmax(s, axis=-1).astype(v.dtype)
    return jnp.einsum('bhqk,bkhd->bqhd', pr, v)


def dwconv(x, w, b):
    n = x.shape[1]
    lo = CONV_W // 2
    xp = jnp.pad(x, ((0, 0), (lo, CONV_W - 1 - lo), (0, 0)))
    y = b
    for k in range(CONV_W):
        y = y + xp[:, k:k + n] * w[k]
    return y


def linear_scan(a, u, h0, reverse):
    edge = -1 if reverse else 0
    u = u.at[:, edge].add(a[:, edge] * h0)

    def combine(left, right):
        a_l, u_l = left
        a_r, u_r = right
        return a_l * a_r, a_r * u_l + u_r

    _, h = lax.associative_scan(combine, (a, u), reverse=reverse, axis=1)
    return h


def rglru_direction(x, w_a, b_a, w_x, b_x, lam, h0, reverse):
    b, n, wdt = x.shape
    xb = x.reshape(b, n, LRU_BLOCKS, LRU_BW)
    r = jax.nn.sigmoid(jnp.einsum('bsnj,njk->bsnk', xb, w_a).reshape(b, n, wdt) + b_a)
    i = jax.nn.sigmoid(jnp.einsum('bsnj,njk->bsnk', xb, w_x).reshape(b, n, wdt) + b_x)
    log_a = -LRU_C * jax.nn.softplus(-lam.astype(jnp.float32)) * r.astype(jnp.float32)
    a = jnp.exp(log_a)
    u = jnp.sqrt(-jnp.expm1(2.0 * log_a)) * (i * x).astype(jnp.float32)
    return linear_scan(a, u, h0, reverse)


def lru_dir(p, d):
    return (p['lru_w_a'][d], p['lru_b_a'][d], p['lru_w_x'][d], p['lru_b_x'][d], p['lru_lambda'][d])


def merge_branches(att, lru, g_att, g_lru, p):
    y_att = att @ p['w_o_attn']
    y_lru = lru @ p['w_o_lru']
    m = (jax.nn.sigmoid(g_att + p['b_branch_gate'][:D_MODEL]) * y_att
         + jax.nn.sigmoid(g_lru + p['b_branch_gate'][D_MODEL:]) * y_lru)
    return m @ p['w_out']


def token_mixer(h, hc, p, cos, sin, need_ctx):
    b, n, _ = h.shape
    c_q, c_kv, k_pe, x_r, g_r, g_att, g_lru = split_cols(h @ p['w_in'])
    cc_q, cc_kv, ck_pe, cx_r, cg_r, cg_att, cg_lru = split_cols(hc @ p['w_in'])
    q = mla_queries(c_q, p, cos, sin)
    k, v = mla_keys_values(c_kv, k_pe, p, cos, sin)
    ck, cv = mla_keys_values(cc_kv, ck_pe, p, None, None)
    k_all = jnp.concatenate([ck, k], axis=1)
    v_all = jnp.concatenate([cv, v], axis=1)
    n_blk = n // Q_BLOCK
    qb = q.reshape(b, n_blk, Q_BLOCK, MLA_HEADS, QK_DIM).transpose(1, 0, 2, 3, 4)
    ob = lax.map(lambda qq: softmax_attend(qq, k_all, v_all), qb)
    att = ob.transpose(1, 0, 2, 3, 4).reshape(b, n, MLA_HEADS * V_HEAD)
    xl = dwconv(x_r, p['conv_w'], p['conv_b'])
    xc = dwconv(cx_r, p['conv_w'], p['conv_b'])
    zeros = jnp.zeros((b, LRU_WIDTH), jnp.float32)
    hc_f = rglru_direction(xc, *lru_dir(p, 0), zeros, False)
    hc_b = rglru_direction(xc, *lru_dir(p, 1), zeros, True)
    h_f = rglru_direction(xl, *lru_dir(p, 0), hc_f[:, -1], False)
    h_b = rglru_direction(xl, *lru_dir(p, 1), hc_b[:, 0], True)
    lru = (h_f + h_b).astype(h.dtype) * jax.nn.gelu(g_r)
    out = merge_branches(att, lru, g_att, g_lru, p)
    if not need_ctx:
        return out, None
    cq = mla_queries(cc_q, p, None, None)
    catt = softmax_attend(cq, ck, cv).reshape(b, -1, MLA_HEADS * V_HEAD)
    clru = (hc_f + hc_b).astype(h.dtype) * jax.nn.gelu(cg_r)
    return out, merge_branches(catt, clru, cg_att, cg_lru, p)


def clamped_swiglu(g, u):
    g = jnp.minimum(g, SWIGLU_LIMIT)
    u = jnp.clip(u, -SWIGLU_LIMIT, SWIGLU_LIMIT)
    return g * jax.nn.sigmoid(SWIGLU_ALPHA * g) * (u + 1)


def moe_ffn(h, p):
    b, n, d = h.shape
    t = b * n
    xf = h.reshape(t, d)
    logits = (xf @ p['w_router'] + p['b_router']).astype(jnp.float32)
    top_val, top_idx = lax.top_k(logits, TOP_K)
    gates = jax.nn.softmax(top_val, axis=-1)
    e_flat = top_idx.reshape(-1)
    tok_flat = jnp.repeat(jnp.arange(t, dtype=jnp.int32), TOP_K)
    w_flat = gates.reshape(-1)
    order = jnp.argsort(e_flat)
    e_sorted = e_flat[order]
    counts = jnp.bincount(e_flat, length=N_EXPERTS)
    padded = ((counts + MOE_BLOCK - 1) // MOE_BLOCK) * MOE_BLOCK
    start = jnp.cumsum(counts) - counts
    pad_end = jnp.cumsum(padded)
    pad_start = pad_end - padded
    n_assign = t * TOP_K
    dest = pad_start[e_sorted] + (jnp.arange(n_assign, dtype=jnp.int32) - start[e_sorted])
    n_blocks = (n_assign + MOE_BLOCK - 1) // MOE_BLOCK + N_EXPERTS
    n_rows = n_blocks * MOE_BLOCK
    row_tok = jnp.full((n_rows,), t, jnp.int32).at[dest].set(tok_flat[order])
    row_w = jnp.zeros((n_rows,), jnp.float32).at[dest].set(w_flat[order])
    blk_start = jnp.arange(n_blocks, dtype=jnp.int32) * MOE_BLOCK
    blk_expert = jnp.minimum(jnp.searchsorted(pad_end, blk_start, side='right'), N_EXPERTS - 1)
    x_pad = jnp.concatenate([xf, jnp.zeros((1, d), xf.dtype)], axis=0)
    rows = x_pad[row_tok].reshape(n_blocks, MOE_BLOCK, d)

    def expert_block(args):
        xb, e = args
        g = xb @ p['w_exp_gate'][e] + p['b_exp_gate'][e]
        u = xb @ p['w_exp_up'][e] + p['b_exp_up'][e]
        return clamped_swiglu(g, u) @ p['w_exp_down'][e] + p['b_exp_down'][e]

    yb = lax.map(expert_block, (rows, blk_expert)).reshape(n_rows, d)
    y = jnp.zeros((t + 1, d), h.dtype).at[row_tok].add(yb * row_w[:, None].astype(h.dtype))
    return y[:t].reshape(b, n, d)


def setup_inputs(seed: int = 0) -> dict:
    key = jax.random.key(seed)
    ks = iter(jax.random.split(key, 40))
    f32 = jnp.float32
    L, D, E = DEPTH, D_MODEL, N_EXPERTS

    def nrm(shape, scale):
        return jax.random.normal(next(ks), shape, f32) * scale

    u = jax.random.uniform(next(ks), (L, 2, LRU_WIDTH), f32, 0.9, 0.999)
    s = u ** (1.0 / LRU_C)
    lam = jnp.log(s) - jnp.log1p(-s)
    return {
        'x': nrm((BATCH, SEQ, D), 1.0),
        'c': nrm((BATCH, D), 1.0),
        'ctx': nrm((BATCH, CTX_LEN, D), 1.0),
        'c_ctx': nrm((D,), 1.0),
        'w_ada': nrm((L, D, 6 * D), 0.5 * D ** -0.5),
        'b_ada': nrm((L, 6 * D), 0.01),
        'norm_mix': 1.0 + nrm((L, D), 0.01),
        'norm_ffn': 1.0 + nrm((L, D), 0.01),
        'w_in': nrm((L, D, D_IN), D ** -0.5),
        'b_branch_gate': nrm((L, 2 * D), 0.01),
        'q_norm': 1.0 + nrm((L, Q_LORA), 0.01),
        'w_uq': nrm((L, Q_LORA, MLA_HEADS * QK_DIM), Q_LORA ** -0.5),
        'kv_norm': 1.0 + nrm((L, KV_LORA), 0.01),
        'w_ukv': nrm((L, KV_LORA, MLA_HEADS * (QK_NOPE + V_HEAD)), KV_LORA ** -0.5),
        'w_o_attn': nrm((L, MLA_HEADS * V_HEAD, D), (MLA_HEADS * V_HEAD) ** -0.5),
        'conv_w': nrm((L, CONV_W, LRU_WIDTH), CONV_W ** -0.5),
        'conv_b': nrm((L, LRU_WIDTH), 0.01),
        'lru_w_a': nrm((L, 2, LRU_BLOCKS, LRU_BW, LRU_BW), LRU_BW ** -0.5),
        'lru_b_a': nrm((L, 2, LRU_WIDTH), 0.01),
        'lru_w_x': nrm((L, 2, LRU_BLOCKS, LRU_BW, LRU_BW), LRU_BW ** -0.5),
        'lru_b_x': nrm((L, 2, LRU_WIDTH), 0.01),
        'lru_lambda': lam,
        'w_o_lru': nrm((L, LRU_WIDTH, D), LRU_WIDTH ** -0.5),
        'w_out': nrm((L, D, D), D ** -0.5),
        'w_router': nrm((L, D, E), D ** -0.5),
        'b_router': nrm((L, E), 0.01),
        'w_exp_gate': nrm((L, E, D, D_FF), D ** -0.5),
        'b_exp_gate': nrm((L, E, D_FF), 0.01),
        'w_exp_up': nrm((L, E, D, D_FF), D ** -0.5),
        'b_exp_up': nrm((L, E, D_FF), 0.01),
        'w_exp_down': nrm((L, E, D_FF, D), D_FF ** -0.5),
        'b_exp_down': nrm((L, E, D), 0.01),
        'final_norm': 1.0 + nrm((D,), 0.01),
    }


def reference(x, c, ctx, c_ctx, w_ada, b_ada, norm_mix, norm_ffn, w_in, b_branch_gate,
              q_norm, w_uq, kv_norm, w_ukv, w_o_attn, conv_w, conv_b, lru_w_a, lru_b_a,
              lru_w_x, lru_b_x, lru_lambda, w_o_lru, w_out, w_router, b_router,
              w_exp_gate, b_exp_gate, w_exp_up, b_exp_up, w_exp_down, b_exp_down, final_norm):
    n_ctx = ctx.shape[1]
    cos, sin = axial_rope_tables(x.shape[1])
    cos = cos.astype(x.dtype)
    sin = sin.astype(x.dtype)
    x_lat, x_ctx = x, ctx
    for l in range(DEPTH):
        need_ctx = l < DEPTH - 1
        p = {
            'w_in': w_in[l], 'b_branch_gate': b_branch_gate[l],
            'q_norm': q_norm[l], 'w_uq': w_uq[l], 'kv_norm': kv_norm[l], 'w_ukv': w_ukv[l],
            'w_o_attn': w_o_attn[l], 'conv_w': conv_w[l], 'conv_b': conv_b[l],
            'lru_w_a': lru_w_a[l], 'lru_b_a': lru_b_a[l], 'lru_w_x': lru_w_x[l],
            'lru_b_x': lru_b_x[l], 'lru_lambda': lru_lambda[l], 'w_o_lru': w_o_lru[l],
            'w_out': w_out[l], 'w_router': w_router[l], 'b_router': b_router[l],
            'w_exp_gate': w_exp_gate[l], 'b_exp_gate': b_exp_gate[l],
            'w_exp_up': w_exp_up[l], 'b_exp_up': b_exp_up[l],
            'w_exp_down': w_exp_down[l], 'b_exp_down': b_exp_down[l],
        }
        sh1, sc1, g1, sh2, sc2, g2 = adaln(c, w_ada[l], b_ada[l])
        csh1, csc1, cg1, csh2, csc2, cg2 = adaln(c_ctx[None, :], w_ada[l], b_ada[l])
        h = modulate(rmsnorm(x_lat, norm_mix[l]), sh1, sc1)
        hc = modulate(rmsnorm(x_ctx, norm_mix[l]), csh1, csc1)
        o, oc = token_mixer(h, hc, p, cos, sin, need_ctx)
        x_lat = x_lat + g1 * o
        h = modulate(rmsnorm(x_lat, norm_ffn[l]), sh2, sc2)
        if need_ctx:
            x_ctx = x_ctx + cg1 * oc
            hc = modulate(rmsnorm(x_ctx, norm_ffn[l]), csh2, csc2)
            y = moe_ffn(jnp.concatenate([hc, h], axis=1), p)
            x_ctx = x_ctx + cg2 * y[:, :n_ctx]
            y = y[:, n_ctx:]
        else:
            y = moe_ffn(h, p)
        x_lat = x_lat + g2 * y
    return rmsnorm(x_lat, final_norm)
```

```python
import contextlib
import numpy as np
import concourse.bass as bass
import concourse.mybir as mybir
from concourse.ap import AP
from concourse.bass_utils import run_bass_kernel_spmd

F32 = mybir.dt.float32
BF16 = mybir.dt.bfloat16
AF = mybir.ActivationFunctionType
ALU = mybir.AluOpType

ENGS = ("pe", "act", "dve", "pool", "sp")
DMA_RING = 8

D = 1024
NL = 2048
NCX = 256
NT = NL + NCX
EPS = 1e-6
ATTN_SCALE = 192 ** -0.5
NE = 32
C_CQ, C_CKV, C_KPE, C_XR, C_GR, C_GATT, C_GLRU = 0, 256, 384, 448, 1472, 2496, 3520
C_KPESW = 4544


class Res:
    __slots__ = ("name", "last_w", "readers")

    def __init__(self, name=""):
        self.name = name
        self.last_w = None
        self.readers = []


class Ins:
    __slots__ = ("eng", "fn", "deps", "is_dma", "flag", "dma_slot", "dma_val")

    def __init__(self, eng, fn, is_dma):
        self.eng = eng
        self.fn = fn
        self.deps = set()
        self.is_dma = is_dma
        self.flag = False


class _Rec:
    def __getattr__(self, name):
        def f(*a, **k):
            self.call = (name, a, k)
            return self
        return f


class Prog:
    def __init__(self, nc):
        self.nc = nc
        self.ins = []
        self.per_eng = {e: [] for e in ENGS}
        self.last_on_eng = {e: None for e in ENGS}
        self.sb_off = 16384
        self.top = 228992
        self.sb_hw = 0
        self.n_alloc = 0
        self._dmas = []

    def sb(self, shape, dtype, name=None):
        esz = 4 if dtype == F32 else 2
        nbytes = int(np.prod(shape[1:])) * esz
        off = (self.sb_off + 63) // 64 * 64
        self.n_alloc += 1
        t = self.nc.alloc_sbuf_tensor_at(name or f"sb{self.n_alloc}", list(shape), dtype, offset=off)
        self.sb_off = off + nbytes
        self.sb_hw = max(self.sb_hw, self.sb_off)
        assert self.sb_off <= self.top, f"SBUF overflow {self.sb_off} > {self.top} ({name})"
        return t

    def sb_top(self, shape, dtype, name=None):
        esz = 4 if dtype == F32 else 2
        nbytes = (int(np.prod(shape[1:])) * esz + 63) // 64 * 64
        self.top -= nbytes
        self.n_alloc += 1
        assert self.top >= self.sb_off
        return self.nc.alloc_sbuf_tensor_at(name or f"sbt{self.n_alloc}", list(shape), dtype, offset=self.top)

    def add(self, eng, fn, reads=(), writes=(), dma=False):
        i = len(self.ins)
        reads = list(dict.fromkeys(reads))
        writes = list(dict.fromkeys(writes))
        rec = _Rec()
        fn(rec)
        name, a, k = rec.call
        ins = Ins(eng, (lambda en, name=name, a=a, k=k: getattr(en, name)(*a, **k)), dma)
        for r in reads:
            if r.last_w is not None:
                ins.deps.add(r.last_w)
        for w in writes:
            if w.last_w is not None:
                ins.deps.add(w.last_w)
            ins.deps.update(w.readers)
        for r in reads:
            if not dma:
                r.readers = [j for j in r.readers if self.ins[j].is_dma or self.ins[j].eng != eng]
            r.readers.append(i)
        for w in writes:
            w.last_w = i
            w.readers = []
        self.per_eng[eng].append(i)
        self.ins.append(ins)
        self.last_on_eng[eng] = i
        if dma:
            self._dmas.append(i)
        return i

    def barrier(self):
        lasts = [v for v in self.last_on_eng.values() if v is not None]
        for e in ENGS:
            ins = Ins(e, None, False)
            ins.deps = set(lasts) | set(self._dmas)
            self.per_eng[e].append(len(self.ins))
            self.ins.append(ins)
        self._dmas = []

    def emit(self, final_waits=()):
        nc = self.nc
        ins = self.ins
        for x in ins:
            nd = set()
            for d in x.deps:
                dx = ins[d]
                if dx.eng == x.eng and not dx.is_dma and x.eng == "pe":
                    continue
                nd.add(d)
            x.deps = nd
        for x in ins:
            for d in x.deps:
                ins[d].flag = True
        sem_cnt = {e: 0 for e in ENGS}
        sig_val = {}
        dma_cnt = {e: 0 for e in ENGS}
        cc_cnt = 0
        has_cc = set()
        for e in ENGS:
            for i in self.per_eng[e]:
                x = ins[i]
                if x.is_dma == "cc":
                    k = cc_cnt
                    cc_cnt += 1
                    x.dma_slot = DMA_RING + (k % 4)
                    x.dma_val = (k // 4 + 1)
                    dma_cnt[e] += 0
                    has_cc.add(e)
                elif x.is_dma:
                    k = dma_cnt[e]
                    dma_cnt[e] += 1
                    x.dma_slot = k % DMA_RING
                    x.dma_val = 16 * (k // DMA_RING + 1)
                elif x.flag:
                    sem_cnt[e] += 1
                    sig_val[i] = sem_cnt[e]
        self.stats = dict(n=len(ins), per_eng={e: len(v) for e, v in self.per_eng.items()},
                          sig=dict(sem_cnt), dma=dict(dma_cnt), sb_hw=self.sb_hw)
        with contextlib.ExitStack() as st:
            esem = {e: st.enter_context(nc.semaphore(f"s_{e}")) for e in ENGS}
            dsem = {e: [st.enter_context(nc.semaphore(f"d_{e}{k}")) for k in range(DMA_RING + (4 if e in has_cc else 0))]
                    for e in ENGS if dma_cnt[e] > 0 or e in has_cc}
            block = st.enter_context(nc.Block())
            engobj = {"pe": block.tensor, "act": block.scalar, "dve": block.vector,
                      "pool": block.gpsimd, "sp": block.sync}

            def make(e):
                def body(eng):
                    waited = {}

                    def wait(sem, key, val):
                        if waited.get(key, 0) >= val:
                            return
                        waited[key] = val
                        eng.wait_ge(sem, val)
                    for i in self.per_eng[e]:
                        x = ins[i]
                        for d in sorted(x.deps):
                            dx = ins[d]
                            if dx.is_dma:
                                wait(dsem[dx.eng][dx.dma_slot], ("d", dx.eng, dx.dma_slot), dx.dma_val)
                            else:
                                wait(esem[dx.eng], ("e", dx.eng), sig_val[d])
                        if x.fn is None:
                            continue
                        if x.is_dma == "cc":
                            if x.dma_val > 1:
                                wait(dsem[e][x.dma_slot], ("d", e, x.dma_slot), x.dma_val - 1)
                            x.fn(eng).then_inc(dsem[e][x.dma_slot], 1)
                        elif x.is_dma:
                            if x.dma_val > 16:
                                wait(dsem[e][x.dma_slot], ("d", e, x.dma_slot), x.dma_val - 16)
                            x.fn(eng).then_inc(dsem[e][x.dma_slot], 16)
                        else:
                            r = x.fn(eng)
                            if x.flag:
                                r.then_inc(esem[e], 1)
                    if e == "sp":
                        for i in final_waits:
                            x = ins[i]
                            wait(dsem[x.eng][x.dma_slot], ("d", x.eng, x.dma_slot), x.dma_val)
                return body
            for e in ENGS:
                engobj[e](make(e))


class Ring:
    def __init__(self, P, n, shape, dtype, name):
        self.bufs = [P.sb(shape, dtype, f"{name}{i}") for i in range(n)]
        self.res = [Res(f"{name}{i}") for i in range(n)]
        self.i = 0

    def next(self):
        k = self.i % len(self.bufs)
        self.i += 1
        return self.bufs[k], self.res[k]


def rev_ap(t_ap, n):
    return AP(t_ap.tensor, t_ap.offset + (n - 1), [list(t_ap.ap[0]), [-1, n]])


def chunks(with_ctx=True):
    cs = [(i * 512, 512, 0) for i in range(4)]
    if with_ctx:
        cs.append((NL, NCX, 1))
    return cs


GROUPS = [[0, 1, 2, 3], [4, 5, 6, 7]]
KVROWS = 320


def build_fused():
    nc = bass.Bass("TRN2", target_bir_lowering=False)
    P = Prog(nc)

    def din(name, shape, dt=F32):
        return nc.dram_tensor(name, list(shape), dt, kind="ExternalInput").ap()

    def dout(name, shape, dt=F32):
        return nc.dram_tensor(name, list(shape), dt, kind="ExternalOutput").ap()

    def dscr(name, shape, dt=F32):
        return nc.dram_tensor(name, list(shape), dt, kind="Internal").ap()

    def A(eng, fn, reads=(), writes=(), dma=False):
        return P.add(eng, fn, reads, writes, dma)

    xin = din("xin", [128, 8, NT])
    xhalo = din("xhalo", [128, 8, 3])
    edge = din("edge", [128, 2])
    cc = din("cc", [128, 8, 2])
    ropeT = din("ropeT", [64, 2, NL])
    ident = din("ident", [128, 128])
    cmask = din("cmask", [128, 4, 2])
    hmask = din("hmask", [128, 4, 2])
    yout = dout("yout", [128, 8, NL])

    ps = [nc.alloc_psum_tensor(f"ps{i}", [128, 512], F32) for i in range(8)]
    psr = [Res(f"ps{i}") for i in range(8)]

    xlat = P.sb([128, 8, NT], F32, "xlat")
    xlat_r = [[Res(f"xl{k}_{c}") for c in range(5)] for k in range(8)]
    ones32 = P.sb([128, 128], F32, "ones32")
    onesbf = P.sb([128, 128], BF16, "onesbf")
    ident_sb = P.sb([128, 128], F32, "ident")
    mod = P.sb([128, 48, 2], F32, "mod")
    modA = P.sb([128, 3, 8, 2], F32, "modA")
    norms_sb = P.sb([128, 3, 8], F32, "norms")
    vecs_sb = P.sb([128, 19], F32, "vecs")
    edge_sb = P.sb([128, 2], F32, "edge")
    r_const = Res("const")
    r_mod = Res("mod")
    G_SH1, G_SC1, G_G1, G_SH2, G_SC2, G_G2 = range(6)

    A("dve", lambda e: e.memset(ones32[:], 1.0), writes=[r_const])
    A("dve", lambda e: e.memset(onesbf[:], 1.0), writes=[r_const])
    A("sp", lambda e: e.dma_start(out=ident_sb[:], in_=ident), writes=[r_const], dma=True)
    A("sp", lambda e: e.dma_start(out=edge_sb[:], in_=edge), writes=[r_const], dma=True)
    for k in range(8):
        for ci, (c0, W, s) in enumerate(chunks()):
            A("sp", lambda e: e.dma_start(out=xlat[:, k, c0:c0 + W], in_=xin[:, k, c0:c0 + W]), writes=[xlat_r[k][ci]], dma=True)
    M0 = P.sb_off

    NH = {}

    def alloc_norm():
        NH["sq"] = Ring(P, 3, [128, 512], F32, "sq")
        NH["rt"] = P.sb([128, 512], F32, "rt")
        NH["rstd"] = P.sb([128, 512], F32, "rstd")
        NH["r"] = Res("rstd")
        NH["tmp"] = Ring(P, 3, [128, 512], F32, "ntmp")

    def norm_mod(ci, c0, W, s, n_i, shift_grp, dsts):
        rt_sb, rstd_sb, r_rstd = NH["rt"], NH["rstd"], NH["r"]
        for k in range(8):
            sq, sqr = NH["sq"].next()
            A("act", lambda e: e.activation(out=sq[:, :W], in_=xlat[:, k, c0:c0 + W], func=AF.Square), reads=[xlat_r[k][ci]], writes=[sqr])
            A("pe", lambda e: e.matmul(ps[7][:, :W], ones32[:], sq[:, :W], start=(k == 0), stop=(k == 7)), reads=[sqr, r_const], writes=[psr[7]])
        A("act", lambda e: e.activation(out=rt_sb[:, :W], in_=ps[7][:, :W], func=AF.Sqrt, scale=1.0 / D, bias=EPS), reads=[psr[7]], writes=[r_rstd])
        A("dve", lambda e: e.reciprocal(out=rstd_sb[:, :W], in_=rt_sb[:, :W]), reads=[r_rstd], writes=[r_rstd])
        for k in range(8):
            tm, tmr = NH["tmp"].next()
            A("dve", lambda e: e.tensor_tensor(out=tm[:, :W], in0=xlat[:, k, c0:c0 + W], in1=rstd_sb[:, :W], op=ALU.mult),
              reads=[xlat_r[k][ci], r_rstd], writes=[tmr])
            bias = mod[:, shift_grp * 8 + k, s:s + 1] if shift_grp is not None else 0.0
            for (dd, dr, dc) in dsts:
                A("act", lambda e: e.activation(out=dd[:, k, dc:dc + W], in_=tm[:, :W], func=AF.Identity, scale=modA[:, n_i, k, s:s + 1], bias=bias),
                  reads=[tmr, r_mod], writes=[dr])

    def feat_rstd(src_list, W, nfeat, out_rstd, out_res):
        rt_sb, r_rstd = NH["rt"], NH["r"]
        n = len(src_list)
        for i, (ap_, r_) in enumerate(src_list):
            sq, sqr = NH["sq"].next()
            A("act", lambda e: e.activation(out=sq[:, :W], in_=ap_, func=AF.Square), reads=[r_], writes=[sqr])
            A("pe", lambda e: e.matmul(ps[7][:, :W], ones32[:], sq[:, :W], start=(i == 0), stop=(i == n - 1)), reads=[sqr, r_const], writes=[psr[7]])
        A("act", lambda e: e.activation(out=rt_sb[:, :W], in_=ps[7][:, :W], func=AF.Sqrt, scale=1.0 / nfeat, bias=EPS), reads=[psr[7]], writes=[r_rstd])
        A("dve", lambda e: e.reciprocal(out=out_rstd[:, :W], in_=rt_sb[:, :W]), reads=[r_rstd], writes=[out_res])


    finals = []
    au_scr = dscr("au_scr", [8, 2, 2, 128, NT], F32)
    r_auscr = Res("auscr")

    def layer(l):
        last = (l == 1)
        sfx = f"_{l}"
        w_ada = din("w_ada" + sfx, [48, 128, 8, 128])
        b_ada = din("b_ada" + sfx, [128, 48])
        norms = din("norms" + sfx, [128, 3, 8])
        w_in = din("w_in" + sfx, [128, 8, 4608])
        vecs = din("vecs" + sfx, [128, 19])
        convp = din("convp" + sfx, [128, 5, 8])
        lrup = din("lrup" + sfx, [128, 3, 2, 8])
        lru_w = din("lru_w" + sfx, [8, 128, 2, 2, 128])
        w_uq = din("w_uq" + sfx, [128, 2, 8, 256])
        w_ukT = din("w_ukT" + sfx, [128, 8, 128])
        w_uv = din("w_uv" + sfx, [128, 8, 128])
        w_oa = din("w_oa" + sfx, [8, 128, 8, 128])
        w_ol = din("w_ol" + sfx, [8, 128, 8, 128])
        w_gatt = din("w_gatt" + sfx, [8, 128, 8, 128])
        w_glru = din("w_glru" + sfx, [8, 128, 8, 128])
        w_out = din("w_out" + sfx, [8, 128, 8, 128])
        w_router = din("w_router" + sfx, [128, 8, NE])
        b_router = din("b_router" + sfx, [128, NE])
        weg = din("weg" + sfx, [NE, 8, 128, 8, 128])
        weu = din("weu" + sfx, [NE, 8, 128, 8, 128])
        wed = din("wed" + sfx, [NE, 8, 128, 8, 128])
        beg = din("beg" + sfx, [128, NE, 8])
        beu = din("beu" + sfx, [128, NE, 8])
        bed = din("bed" + sfx, [NE, D])
        lru_scr = dscr("lru_scr" + sfx, [128, 8, NT], BF16)
        att_scr = dscr("att_scr" + sfx, [128, 8, NT], BF16)
        gt_scr = dscr("gt_scr" + sfx, [NE, NT], F32)
        kv_bnc = dscr("kv_bnc" + sfx, [256, NL], BF16)
        kv_gat = dscr("kv_gat" + sfx, [4 * 256, NL], BF16)
        kp_bnc = dscr("kp_bnc" + sfx, [128, NL], BF16)
        kp_gat = dscr("kp_gat" + sfx, [4 * 128, NL], BF16)
        sm_bnc = dscr("sm_bnc" + sfx, [128, 32], F32)
        sm_gat = dscr("sm_gat" + sfx, [512, 32], F32)
        hl_bnc = dscr("hl_bnc" + sfx, [128, 24], F32)
        hl_gat = dscr("hl_gat" + sfx, [512, 24], F32)
        r_kvb, r_kvg, r_smb, r_smg, r_hlb, r_hlg, r_kpb, r_kpg = (Res(n) for n in ("kvb", "kvg", "smb", "smg", "hlb", "hlg", "kpb", "kpg"))
        A("sp", lambda e: e.dma_start(out=norms_sb[:], in_=norms), writes=[r_const], dma=True)
        A("sp", lambda e: e.dma_start(out=vecs_sb[:], in_=vecs), writes=[r_const], dma=True)
        cc_sb = P.sb([128, 8, 2], F32, "cc")
        sil = P.sb([128, 8, 2], F32, "sil")
        bada_sb = P.sb([128, 48], F32, "bada")
        r_cc = Res("cc")
        A("sp", lambda e: e.dma_start(out=cc_sb[:], in_=cc), writes=[r_cc], dma=True)
        A("sp", lambda e: e.dma_start(out=bada_sb[:], in_=b_ada), writes=[r_cc], dma=True)
        A("act", lambda e: e.activation(out=sil[:], in_=cc_sb[:], func=AF.Silu), reads=[r_cc], writes=[r_cc])
        wring = Ring(P, 4, [128, 8, 128], F32, "wada")
        for j in range(48):
            wt, wr = wring.next()
            A("sp", lambda e: e.dma_start(out=wt[:], in_=w_ada[j]), writes=[wr], dma=True)
            for k in range(8):
                A("pe", lambda e: e.matmul(ps[0][:, 2 * j:2 * j + 2], wt[:, k, :], sil[:, k, :], start=(k == 0), stop=(k == 7)),
                  reads=[wr, r_cc], writes=[psr[0]])
        for s in range(2):
            A("dve", lambda e: e.tensor_tensor(out=mod[:, :, s], in0=ps[0][:, s:96:2], in1=bada_sb[:], op=ALU.add),
              reads=[psr[0], r_cc], writes=[r_mod])
        for n_i, grp in ((0, G_SC1), (1, G_SC2)):
            for s in range(2):
                A("dve", lambda e: e.scalar_tensor_tensor(out=modA[:, n_i, :, s], in0=mod[:, grp * 8:(grp + 1) * 8, s], scalar=1.0,
                                                          in1=norms_sb[:, n_i, :], op0=ALU.add, op1=ALU.mult),
                  reads=[r_mod, r_const], writes=[r_mod])
        for s in range(2):
            A("dve", lambda e: e.tensor_copy(out=modA[:, 2, :, s], in_=norms_sb[:, 2, :]), reads=[r_const], writes=[r_mod])
        P.barrier()
        P.sb_off = M0

        wiring = Ring(P, 3, [128, 8, 128], BF16, "wi")
        wiring.bufs = [P.sb_top([128, 8, 128], BF16, f"wiT{i}") for i in range(3)]
        P.sb_off = M0
        cqn = P.sb_top([128, 2, NT], BF16, "cqn")
        r_cqn = [Res(f"cqn{c}") for c in range(5)]
        ckv32c = P.sb_top([128, NCX], F32, "ckv32c")
        kpe32c = P.sb_top([64, NCX], F32, "kpe32c")
        r_ckv32, r_kpe32 = Res("ckv32c"), Res("kpe32c")
        TOP_KEEP = P.top

        def load_wi(col0, ncols=128):
            wt, wr = wiring.next()
            A("pool", lambda e: e.dma_start(out=wt[:, :, 0:ncols], in_=w_in[:, :, col0:col0 + ncols]), writes=[wr], dma=True)
            return wt, wr

        h = P.sb([128, 8, NT + 3], BF16, "h")
        h_r = [Res(f"h{c}") for c in range(6)]
        M1 = P.sb_off
        alloc_norm()
        for ci, (c0, W, s) in enumerate(chunks()):
            norm_mod(ci, c0, W, s, 0, G_SH1, [(h, h_r[ci], c0)])
        halo32 = P.sb([128, 8, 4], F32, "halo32")
        hsq = P.sb([128, 8, 4], F32, "hsq")
        r_halo = Res("halo")
        if l == 0:
            A("sp", lambda e: e.dma_start(out=halo32[:, :, 0:3], in_=xhalo), writes=[r_halo], dma=True)
        else:
            hb_sb = P.sb([128, 8, 3], F32, "hb_sb")
            hg_sb = P.sb([128, 4, 8, 3], F32, "hg_sb")
            hm_sb = P.sb([128, 4, 2], F32, "hm_sb")
            r_hb = Res("hb")
            A("sp", lambda e: e.dma_start(out=hm_sb[:], in_=hmask), writes=[r_hb], dma=True)
            for k in range(8):
                A("dve", lambda e: e.tensor_copy(out=hb_sb[:, k, 0:1], in_=xlat[:, k, 0:1]), reads=[xlat_r[k][0]], writes=[r_hb])
                A("dve", lambda e: e.tensor_copy(out=hb_sb[:, k, 1:3], in_=xlat[:, k, NL - 2:NL]), reads=[xlat_r[k][3]], writes=[r_hb])
            A("sp", lambda e: e.dma_start(out=hl_bnc.rearrange("p (k c) -> p k c", c=3), in_=hb_sb[:]), reads=[r_hb], writes=[r_hlb], dma=True)
            A("pool", lambda e: e.collective_compute("AllGather", ALU.bypass, replica_groups=GROUPS, ins=[hl_bnc], outs=[hl_gat]),
              reads=[r_hlb], writes=[r_hlg], dma="cc")
            A("sp", lambda e: e.dma_start(out=hg_sb[:], in_=hl_gat.rearrange("(r p) (k c) -> p r k c", p=128, c=3)), reads=[r_hlg], writes=[r_hb], dma=True)
            A("dve", lambda e: e.memset(halo32[:], 0.0), writes=[r_halo])
            for r in range(4):
                A("dve", lambda e: e.scalar_tensor_tensor(out=halo32[:, :, 0:2], in0=hg_sb[:, r, :, 1:3], scalar=hm_sb[:, r, 0:1], in1=halo32[:, :, 0:2],
                                                          op0=ALU.mult, op1=ALU.add), reads=[r_hb, r_halo], writes=[r_halo])
                A("dve", lambda e: e.scalar_tensor_tensor(out=halo32[:, :, 2:3], in0=hg_sb[:, r, :, 0:1], scalar=hm_sb[:, r, 1:2], in1=halo32[:, :, 2:3],
                                                          op0=ALU.mult, op1=ALU.add), reads=[r_hb, r_halo], writes=[r_halo])
        A("act", lambda e: e.activation(out=hsq[:, :, 0:3], in_=halo32[:, :, 0:3], func=AF.Square), reads=[r_halo], writes=[r_halo])
        for k in range(8):
            A("pe", lambda e: e.matmul(ps[7][:, 0:3], ones32[:], hsq[:, k, 0:3], start=(k == 0), stop=(k == 7)), reads=[r_halo, r_const], writes=[psr[7]])
        A("act", lambda e: e.activation(out=NH["rt"][:, 0:3], in_=ps[7][:, 0:3], func=AF.Sqrt, scale=1.0 / D, bias=EPS), reads=[psr[7]], writes=[NH["r"]])
        A("dve", lambda e: e.reciprocal(out=NH["rstd"][:, 0:3], in_=NH["rt"][:, 0:3]), reads=[NH["r"]], writes=[NH["r"]])
        for k in range(8):
            A("dve", lambda e: e.tensor_tensor(out=hsq[:, k, 0:3], in0=halo32[:, k, 0:3], in1=NH["rstd"][:, 0:3], op=ALU.mult),
              reads=[r_halo, NH["r"]], writes=[r_halo])
            A("act", lambda e: e.activation(out=h[:, k, NT:NT + 3], in_=hsq[:, k, 0:3], func=AF.Identity, scale=modA[:, 0, k, 0:1],
                                            bias=mod[:, G_SH1 * 8 + k, 0:1]), reads=[r_halo, r_mod], writes=[h_r[5]])

        def proj(wt, wr, M, psb, ci, c0, W):
            for k in range(8):
                A("pe", lambda e: e.matmul(ps[psb][0:M, :W], wt[:, k, 0:M], h[:, k, c0:c0 + W], start=(k == 0), stop=(k == 7)),
                  reads=[wr, h_r[ci]], writes=[psr[psb]])

        def kv_latents(chunk_list, ckvn_dst, ckvn_res, kpe_dst, kpe_res, rope_sb):
            w_ckv, r_ckv = load_wi(C_CKV)
            w_kpe, r_kpe = load_wi(C_KPE, 64)
            if any(s_ == 0 for (_, _, _, s_) in chunk_list):
                w_ksw, r_ksw = load_wi(C_KPESW, 64)
            krst = P.sb([128, 512], F32, "krst")
            r_krst = Res("krst")
            kt1 = P.sb([64, 512], F32, "kt1")
            kt2 = P.sb([64, 512], F32, "kt2")
            r_kt = Res("kt")
            for (ci, c0, W, s) in chunk_list:
                d0 = c0 if s == 0 else 0
                proj(w_ckv, r_ckv, 128, 0, ci, c0, W)
                feat_rstd([(ps[0][:, :W], psr[0])], W, 128, krst, r_krst)
                A("dve", lambda e: e.tensor_tensor(out=krst[:, :W], in0=ps[0][:, :W], in1=krst[:, :W], op=ALU.mult), reads=[psr[0], r_krst], writes=[r_krst])
                A("dve", lambda e: e.tensor_scalar(out=ckvn_dst[:, d0:d0 + W], in0=krst[:, :W], scalar1=vecs_sb[:, 18:19], scalar2=None, op0=ALU.mult),
                  reads=[r_krst, r_const], writes=[ckvn_res])
                proj(w_kpe, r_kpe, 64, 2, ci, c0, W)
                if s == 0:
                    proj(w_ksw, r_ksw, 64, 3, ci, c0, W)
                    A("dve", lambda e: e.tensor_tensor(out=kt1[:, :W], in0=ps[2][0:64, :W], in1=rope_sb[:, 0, c0:c0 + W], op=ALU.mult),
                      reads=[psr[2], r_const], writes=[r_kt])
                    A("dve", lambda e: e.tensor_tensor(out=kt2[:, :W], in0=ps[3][0:64, :W], in1=rope_sb[:, 1, c0:c0 + W], op=ALU.mult),
                      reads=[psr[3], r_const, r_kt], writes=[r_kt])
                    A("dve", lambda e: e.tensor_tensor(out=kpe_dst[0:64, d0:d0 + W], in0=kt1[:, :W], in1=kt2[:, :W], op=ALU.add), reads=[r_kt], writes=[kpe_res])
                else:
                    A("act", lambda e: e.activation(out=kpe_dst[0:64, d0:d0 + W], in_=ps[2][0:64, :W], func=AF.Copy), reads=[psr[2]], writes=[kpe_res])

        r_lruscr = Res("lruscr")
        r_attscr = Res("attscr")

        def lru_stage(pass2):
            m_l = P.sb_off
            convp_sb = P.sb([128, 5, 8], F32, "convp")
            lrup_sb = P.sb([128, 3, 2, 8], F32, "lrup")
            clam = P.sb([128, 2, 2, 8], F32, "clam")
            r_lp = Res("lrup")
            A("sp", lambda e: e.dma_start(out=convp_sb[:], in_=convp), writes=[r_lp], dma=True)
            A("sp", lambda e: e.dma_start(out=lrup_sb[:], in_=lrup), writes=[r_lp], dma=True)
            A("act", lambda e: e.activation(out=clam[:, 0], in_=lrup_sb[:, 2], func=AF.Exp, scale=-1.0), reads=[r_lp], writes=[r_lp])
            A("act", lambda e: e.activation(out=clam[:, 0], in_=clam[:, 0], func=AF.Ln, bias=1.0), reads=[r_lp], writes=[r_lp])
            A("dve", lambda e: e.tensor_scalar(out=clam[:, 1], in0=clam[:, 0], scalar1=-16.0, scalar2=None, op0=ALU.mult), reads=[r_lp], writes=[r_lp])
            A("dve", lambda e: e.tensor_scalar(out=clam[:, 0], in0=clam[:, 0], scalar1=-8.0, scalar2=None, op0=ALU.mult), reads=[r_lp], writes=[r_lp])
            lw_ring = Ring(P, 2, [128, 2, 2, 128], BF16, "lruw")
            r_xr, r_xl = Res("xr"), Res("xl")
            if not pass2:
                xr_l = P.sb([128, NL + 4], F32, "xr_l")
                xr_c = P.sb([128, NCX + 4], F32, "xr_c")
                xl32 = P.sb([128, NT], F32, "xl32")
                xlbf = P.sb([128, NT], BF16, "xlbf")
                a_sb = P.sb([128, NT], F32, "a_sb")
                u_sb = P.sb([128, NT], F32, "u_sb")
                r_a = [Res(f"a{c}") for c in range(5)]
                r_u = [Res(f"u{c}") for c in range(5)]
            else:
                a_ring = Ring(P, 2, [128, NT], F32, "a_rg")
                u_ring = Ring(P, 2, [128, NT], F32, "u_rg")
            t_r = Ring(P, 2, [128, 512], F32, "lt_r")
            t_i = Ring(P, 2, [128, 512], F32, "lt_i")
            t_s = Ring(P, 2, [128, 512], F32, "lt_s")
            hd = [P.sb([128, NT], F32, f"hd{d}") for d in range(2)]
            r_hd = [Res("hd0"), Res("hd1")]
            sumr = P.sb([128, 2, 8], F32, "sumr")
            r_sumr = Res("sumr")
            summ_sb = P.sb([128, 8, 4], F32, "summ")
            r_summ = Res("summ")
            carry = P.sb([128, 2, 4], F32, "carry")
            r_carry = Res("carry")
            if pass2:
                summall_sb = P.sb([128, 4, 8, 4], F32, "summall")
                cmask_sb = P.sb([128, 4, 2], F32, "cmask")
                r_sa = Res("summall")
                A("sp", lambda e: e.dma_start(out=summall_sb[:], in_=sm_gat.rearrange("(r p) (k c) -> p r k c", p=128, c=4)), reads=[r_smg], writes=[r_sa], dma=True)
                A("sp", lambda e: e.dma_start(out=cmask_sb[:], in_=cmask), writes=[r_sa], dma=True)
                lru_o = P.sb([128, NT], BF16, "lru_o")
                r_lo = Res("lru_o")
            if not pass2:
                A("dve", lambda e: e.memset(xr_c[:], 0.0), writes=[r_xr])
            else:
                def au_load(c_, d_):
                    ab, ar = a_ring.next()
                    ub, ur = u_ring.next()
                    A("sp", lambda e: e.dma_start(out=ab[:], in_=au_scr[c_, d_, 0]), reads=[r_auscr], writes=[ar], dma=True)
                    A("sp", lambda e: e.dma_start(out=ub[:], in_=au_scr[c_, d_, 1]), reads=[r_auscr], writes=[ur], dma=True)
                    return ab, ar, ub, ur
                au_next = [au_load(0, 0)]
            for c in range(8):
                if not pass2:
                    lw, lwr = lw_ring.next()
                    A("pool", lambda e: e.dma_start(out=lw[:], in_=lru_w[c]), writes=[lwr], dma=True)
                    wt, wr = load_wi(C_XR + c * 128)
                    for (c0, W, s) in chunks():
                        ci = 4 if s else c0 // 512
                        pb = ci % 2
                        proj(wt, wr, 128, pb, ci, c0, W)
                        if s == 0:
                            A("act", lambda e: e.activation(out=xr_l[:, 2 + c0:2 + c0 + W], in_=ps[pb][:, :W], func=AF.Copy), reads=[psr[pb]], writes=[r_xr])
                        else:
                            A("act", lambda e: e.activation(out=xr_c[:, 2:2 + W], in_=ps[pb][:, :W], func=AF.Copy), reads=[psr[pb]], writes=[r_xr])
                    proj(wt, wr, 128, 6, 5, NT, 3)
                    A("dve", lambda e: e.tensor_scalar(out=xr_l[:, 0:2], in0=ps[6][:, 0:2], scalar1=edge_sb[:, 0:1], scalar2=None, op0=ALU.mult),
                      reads=[psr[6], r_const], writes=[r_xr])
                    A("dve", lambda e: e.tensor_scalar(out=xr_l[:, NL + 2:NL + 3], in0=ps[6][:, 2:3], scalar1=edge_sb[:, 1:2], scalar2=None, op0=ALU.mult),
                      reads=[psr[6], r_const], writes=[r_xr])
                if pass2:
                    wg, wgr = load_wi(C_GR + c * 128)
                if not pass2:
                    for (src, n, d0) in ((xr_l, NL, 0), (xr_c, NCX, NL)):
                        A("dve", lambda e: e.tensor_scalar(out=xl32[:, d0:d0 + n], in0=src[:, 0:n], scalar1=convp_sb[:, 0, c:c + 1],
                                                           scalar2=convp_sb[:, 4, c:c + 1], op0=ALU.mult, op1=ALU.add), reads=[r_xr, r_lp], writes=[r_xl])
                        for tap in (1, 2, 3):
                            A("dve", lambda e: e.scalar_tensor_tensor(out=xl32[:, d0:d0 + n], in0=src[:, tap:tap + n], scalar=convp_sb[:, tap, c:c + 1],
                                                                      in1=xl32[:, d0:d0 + n], op0=ALU.mult, op1=ALU.add), reads=[r_xr, r_lp, r_xl], writes=[r_xl])
                    A("act", lambda e: e.activation(out=xlbf[:], in_=xl32[:], func=AF.Copy), reads=[r_xl], writes=[r_xl])
                for d in range(2):
                    if pass2:
                        a_sb, ra_, u_sb, ru_ = au_next[0]
                        r_a = [ra_] * 5
                        r_u = [ru_] * 5
                        nxt = c * 2 + d + 1
                        if nxt < 16:
                            au_next[0] = au_load(nxt // 2, nxt % 2)
                    else:
                        for (c0, W, s) in chunks():
                            ci = 4 if s else c0 // 512
                            A("pe", lambda e: e.matmul(ps[2][:, :W], lw[:, 0, d, :], xlbf[:, c0:c0 + W], start=True, stop=True), reads=[lwr, r_xl], writes=[psr[2]])
                            A("pe", lambda e: e.matmul(ps[3][:, :W], lw[:, 1, d, :], xlbf[:, c0:c0 + W], start=True, stop=True), reads=[lwr, r_xl], writes=[psr[3]])
                            tr, trr = t_r.next()
                            ti, tir = t_i.next()
                            ts, tsr = t_s.next()
                            A("act", lambda e: e.activation(out=tr[:, :W], in_=ps[2][:, :W], func=AF.Sigmoid, bias=lrup_sb[:, 0, d, c:c + 1]),
                              reads=[psr[2], r_lp], writes=[trr])
                            A("act", lambda e: e.activation(out=ti[:, :W], in_=ps[3][:, :W], func=AF.Sigmoid, bias=lrup_sb[:, 1, d, c:c + 1]),
                              reads=[psr[3], r_lp], writes=[tir])
                            A("act", lambda e: e.activation(out=a_sb[:, c0:c0 + W], in_=tr[:, :W], func=AF.Exp, scale=clam[:, 0, d, c:c + 1]),
                              reads=[trr, r_lp], writes=[r_a[ci]])
                            A("act", lambda e: e.activation(out=ts[:, :W], in_=tr[:, :W], func=AF.Exp, scale=clam[:, 1, d, c:c + 1]),
                              reads=[trr, r_lp], writes=[tsr])
                            A("act", lambda e: e.activation(out=ts[:, :W], in_=ts[:, :W], func=AF.Sqrt, scale=-1.0, bias=1.0), reads=[tsr], writes=[tsr])
                            A("dve", lambda e: e.tensor_tensor(out=ti[:, :W], in0=ti[:, :W], in1=xl32[:, c0:c0 + W], op=ALU.mult), reads=[tir, r_xl], writes=[tir])
                            A("dve", lambda e: e.tensor_tensor(out=u_sb[:, c0:c0 + W], in0=ti[:, :W], in1=ts[:, :W], op=ALU.mult), reads=[tir, tsr], writes=[r_u[ci]])
                            if not pass2 and s == 0:
                                A("dve", lambda e: e.tensor_reduce(out=carry[:, d, ci:ci + 1], in_=tr[:, :W], axis=mybir.AxisListType.X, op=ALU.add),
                                  reads=[trr], writes=[r_carry])
                        A("sp", lambda e: e.dma_start(out=au_scr[c, d, 0], in_=a_sb[:]), reads=r_a, writes=[r_auscr], dma=True)
                        A("sp", lambda e: e.dma_start(out=au_scr[c, d, 1], in_=u_sb[:]), reads=r_u, writes=[r_auscr], dma=True)
                    cx = slice(NL, NT)
                    lx = slice(0, NL)
                    if d == 0:
                        A("dve", lambda e: e.tensor_tensor_scan(out=hd[0][:, cx], data0=a_sb[:, cx], data1=u_sb[:, cx], initial=0.0,
                                                                op0=ALU.mult, op1=ALU.add), reads=[r_a[4], r_u[4]], writes=[r_hd[0]])
                    else:
                        A("dve", lambda e: e.tensor_tensor_scan(out=rev_ap(hd[1][:, cx], NCX), data0=rev_ap(a_sb[:, cx], NCX), data1=rev_ap(u_sb[:, cx], NCX),
                                                                initial=0.0, op0=ALU.mult, op1=ALU.add), reads=[r_a[4], r_u[4]], writes=[r_hd[1]])
                    if not pass2:
                        if d == 0:
                            A("dve", lambda e: e.tensor_tensor_scan(out=hd[0][:, lx], data0=a_sb[:, lx], data1=u_sb[:, lx], initial=0.0,
                                                                    op0=ALU.mult, op1=ALU.add), reads=r_a[:4] + r_u[:4], writes=[r_hd[0]])
                            A("dve", lambda e: e.tensor_copy(out=summ_sb[:, c, 1:2], in_=hd[0][:, NL - 1:NL]), reads=[r_hd[0]], writes=[r_summ])
                        else:
                            A("dve", lambda e: e.tensor_tensor_scan(out=rev_ap(hd[1][:, lx], NL), data0=rev_ap(a_sb[:, lx], NL), data1=rev_ap(u_sb[:, lx], NL),
                                                                    initial=0.0, op0=ALU.mult, op1=ALU.add), reads=r_a[:4] + r_u[:4], writes=[r_hd[1]])
                            A("dve", lambda e: e.tensor_copy(out=summ_sb[:, c, 3:4], in_=hd[1][:, 0:1]), reads=[r_hd[1]], writes=[r_summ])
                        A("dve", lambda e: e.tensor_reduce(out=sumr[:, d, c:c + 1], in_=carry[:, d, 0:4], axis=mybir.AxisListType.X, op=ALU.add),
                          reads=[r_carry], writes=[r_sumr])
                        A("act", lambda e: e.activation(out=summ_sb[:, c, 2 * d:2 * d + 1], in_=sumr[:, d, c:c + 1], func=AF.Exp, scale=clam[:, 0, d, c:c + 1]),
                          reads=[r_sumr, r_lp], writes=[r_summ])
                    else:
                        cv = carry[:, d, 0:1]
                        tv = carry[:, d, 1:2]
                        src_col = (NT - 1) if d == 0 else NL
                        A("dve", lambda e: e.tensor_copy(out=cv, in_=hd[d][:, src_col:src_col + 1]), reads=[r_hd[d]], writes=[r_carry])
                        order = range(4) if d == 0 else range(3, -1, -1)
                        for kk in order:
                            A("dve", lambda e: e.scalar_tensor_tensor(out=tv, in0=cv, scalar=summall_sb[:, kk, c, 2 * d:2 * d + 1],
                                                                      in1=summall_sb[:, kk, c, 2 * d + 1:2 * d + 2], op0=ALU.mult, op1=ALU.add),
                              reads=[r_carry, r_sa], writes=[r_carry])
                            A("dve", lambda e: e.tensor_tensor(out=tv, in0=tv, in1=cv, op=ALU.subtract), reads=[r_carry], writes=[r_carry])
                            A("dve", lambda e: e.scalar_tensor_tensor(out=cv, in0=tv, scalar=cmask_sb[:, kk, d:d + 1], in1=cv, op0=ALU.mult, op1=ALU.add),
                              reads=[r_carry, r_sa], writes=[r_carry])
                        if d == 0:
                            A("dve", lambda e: e.tensor_tensor_scan(out=hd[0][:, lx], data0=a_sb[:, lx], data1=u_sb[:, lx], initial=cv,
                                                                    op0=ALU.mult, op1=ALU.add), reads=r_a[:4] + r_u[:4] + [r_carry], writes=[r_hd[0]])
                        else:
                            A("dve", lambda e: e.tensor_tensor_scan(out=rev_ap(hd[1][:, lx], NL), data0=rev_ap(a_sb[:, lx], NL), data1=rev_ap(u_sb[:, lx], NL),
                                                                    initial=cv, op0=ALU.mult, op1=ALU.add), reads=r_a[:4] + r_u[:4] + [r_carry], writes=[r_hd[1]])
                if pass2:
                    A("dve", lambda e: e.tensor_tensor(out=hd[0][:], in0=hd[0][:], in1=hd[1][:], op=ALU.add), reads=r_hd, writes=[r_hd[0]])
                    for (c0, W, s) in chunks():
                        ci = 4 if s else c0 // 512
                        x2, x2r = t_r.next()
                        gi, gir = t_i.next()
                        gs, gsr = t_s.next()
                        proj(wg, wgr, 128, 4, ci, c0, W)
                        A("act", lambda e: e.activation(out=x2[:, :W], in_=ps[4][:, :W], func=AF.Square), reads=[psr[4]], writes=[x2r])
                        A("dve", lambda e: e.tensor_scalar(out=x2[:, :W], in0=x2[:, :W], scalar1=0.044715, scalar2=1.0, op0=ALU.mult, op1=ALU.add),
                          reads=[x2r], writes=[x2r])
                        A("dve", lambda e: e.tensor_tensor(out=gi[:, :W], in0=x2[:, :W], in1=ps[4][:, :W], op=ALU.mult), reads=[x2r, psr[4]], writes=[gir])
                        A("act", lambda e: e.activation(out=gs[:, :W], in_=gi[:, :W], func=AF.Sigmoid, scale=1.5957691216057308), reads=[gir], writes=[gsr])
                        A("dve", lambda e: e.tensor_tensor(out=gs[:, :W], in0=gs[:, :W], in1=ps[4][:, :W], op=ALU.mult), reads=[gsr, psr[4]], writes=[gsr])
                        A("dve", lambda e: e.tensor_tensor(out=lru_o[:, c0:c0 + W], in0=gs[:, :W], in1=hd[0][:, c0:c0 + W], op=ALU.mult),
                          reads=[gsr, r_hd[0]], writes=[r_lo])
                    A("sp", lambda e: e.dma_start(out=lru_scr[:, c, :], in_=lru_o[:]), reads=[r_lo], writes=[r_lruscr], dma=True)
            ret = None
            if not pass2:
                A("sp", lambda e: e.dma_start(out=sm_bnc.rearrange("p (k c) -> p k c", c=4), in_=summ_sb[:]), reads=[r_summ], writes=[r_smb], dma=True)
                A("pool", lambda e: e.collective_compute("AllGather", ALU.bypass, replica_groups=GROUPS, ins=[sm_bnc], outs=[sm_gat]),
                  reads=[r_smb], writes=[r_smg], dma="cc")
            P.barrier()
            P.sb_off = m_l
            return ret

        m_kv = P.sb_off
        rope_sb = P.sb([64, 2, NL], F32, "rope")
        A("sp", lambda e: e.dma_start(out=rope_sb[:], in_=ropeT), writes=[r_const], dma=True)
        kvo = P.sb([128, NL], F32, "kvo")
        kvb = P.sb([128, NL], BF16, "kvb")
        kpb = P.sb([64, NL], BF16, "kpb")
        tokb = P.sb([128, 16, 128], BF16, "tokb")
        r_kvo, r_kpo, r_kvb2, r_tokb = Res("kvo"), Res("kpo"), Res("kvb2"), Res("tokb")
        kv_latents([(ci, c0, W, s) for ci, (c0, W, s) in enumerate(chunks(False))], kvo, r_kvo, kpb, r_kpo, rope_sb)
        A("act", lambda e: e.activation(out=kvb[:], in_=kvo[:], func=AF.Copy), reads=[r_kvo], writes=[r_kvb2])
        for t4 in range(4):
            pb = 4 + (t4 % 2)
            for t in range(4):
                tt = t4 * 4 + t
                A("pe", lambda e: e.transpose(out=ps[pb][:, t * 128:(t + 1) * 128], in_=kvo[:, tt * 128:(tt + 1) * 128], identity=ident_sb[:]),
                  reads=[r_kvo, r_const], writes=[psr[pb]])
            for t in range(4):
                A("act", lambda e: e.activation(out=tokb[:, t4 * 4 + t, :], in_=ps[pb][:, t * 128:(t + 1) * 128], func=AF.Copy),
                  reads=[psr[pb]], writes=[r_tokb])
        A("sp", lambda e: e.dma_start(out=kv_bnc[0:128, :], in_=kvb[:]), reads=[r_kvb2], writes=[r_kvb], dma=True)
        A("sp", lambda e: e.dma_start(out=kp_bnc[0:64, :], in_=kpb[:]), reads=[r_kpo], writes=[r_kpb], dma=True)
        A("sp", lambda e: e.dma_start(out=kp_bnc[64:128, :], in_=kpb[:]), reads=[r_kpo], writes=[r_kpb], dma=True)
        A("sp", lambda e: e.dma_start(out=kv_bnc[128:256, :].rearrange("p (a b) -> p a b", b=128), in_=tokb[:]), reads=[r_tokb], writes=[r_kvb], dma=True)
        A("pool", lambda e: e.collective_compute("AllGather", ALU.bypass, replica_groups=GROUPS, ins=[kv_bnc], outs=[kv_gat]),
          reads=[r_kvb], writes=[r_kvg], dma="cc")
        A("pool", lambda e: e.collective_compute("AllGather", ALU.bypass, replica_groups=GROUPS, ins=[kp_bnc], outs=[kp_gat]),
          reads=[r_kpb], writes=[r_kpg], dma="cc")
        P.barrier()
        P.sb_off = m_kv
        qchunks = list(enumerate(chunks(not last)))
        qrst = P.sb([128, 512], F32, "qrst")
        r_qrst = Res("qrst")
        wq0, wq0r = load_wi(C_CQ)
        wq1, wq1r = load_wi(C_CQ + 128)
        for ci, (c0, W, s) in qchunks:
            proj(wq0, wq0r, 128, 0, ci, c0, W)
            proj(wq1, wq1r, 128, 1, ci, c0, W)
            feat_rstd([(ps[0][:, :W], psr[0]), (ps[1][:, :W], psr[1])], W, 256, qrst, r_qrst)
            for t in range(2):
                tm, tmr = NH["tmp"].next()
                A("dve", lambda e: e.tensor_tensor(out=tm[:, :W], in0=ps[t][:, :W], in1=qrst[:, :W], op=ALU.mult), reads=[psr[t], r_qrst], writes=[tmr])
                A("dve", lambda e: e.tensor_scalar(out=cqn[:, t, c0:c0 + W], in0=tm[:, :W], scalar1=vecs_sb[:, 16 + t:17 + t], scalar2=None, op0=ALU.mult),
                  reads=[tmr, r_const], writes=[r_cqn[ci]])
        kv_latents([(4, NL, NCX, 1)], ckv32c, r_ckv32, kpe32c, r_kpe32, None)
        P.barrier()
        P.sb_off = M1
        lru_stage(False)
        lru_stage(True)
        P.sb_off = M0

        NK = NCX + 4 * NL
        rope_sb = P.sb([64, 2, NL], F32, "rope")
        A("sp", lambda e: e.dma_start(out=rope_sb[:], in_=ropeT), writes=[r_const], dma=True)
        ckvT = P.sb([128, NK], BF16, "ckvT")
        kpeT = P.sb([128, NK], BF16, "kpeT")
        ckvtok = P.sb([128, 66, 128], BF16, "ckvtok")
        r_kv = Res("kvall")
        for q in range(4):
            rb = q * 256
            A("sp", lambda e: e.dma_start(out=ckvT[:, NCX + q * NL:NCX + (q + 1) * NL], in_=kv_gat[rb:rb + 128, :]), reads=[r_kvg], writes=[r_kv], dma=True)
            A("sp", lambda e: e.dma_start(out=kpeT[0:64, NCX + q * NL:NCX + (q + 1) * NL], in_=kp_gat[q * 128:q * 128 + 64, :]), reads=[r_kpg], writes=[r_kv], dma=True)
            A("sp", lambda e: e.dma_start(out=ckvtok[:, 2 + 16 * q:2 + 16 * (q + 1), :], in_=kv_gat[rb + 128:rb + 256, :].rearrange("p (a b) -> p a b", b=128)),
              reads=[r_kvg], writes=[r_kv], dma=True)
        A("act", lambda e: e.activation(out=ckvT[:, 0:NCX], in_=ckv32c[:], func=AF.Copy), reads=[r_ckv32], writes=[r_kv])
        A("dve", lambda e: e.memset(kpeT[64:128, :], 0.0), writes=[r_kv])
        A("act", lambda e: e.activation(out=kpeT[0:64, 0:NCX], in_=kpe32c[:], func=AF.Copy), reads=[r_kpe32], writes=[r_kv])
        for t in range(2):
            A("pe", lambda e: e.transpose(out=ps[4][:, t * 128:(t + 1) * 128], in_=ckv32c[:, t * 128:(t + 1) * 128], identity=ident_sb[:]),
              reads=[r_ckv32, r_const], writes=[psr[4]])
        for t in range(2):
            A("act", lambda e: e.activation(out=ckvtok[:, t, :], in_=ps[4][:, t * 128:(t + 1) * 128], func=AF.Copy), reads=[psr[4]], writes=[r_kv])
        wuq_sb = P.sb([128, 2, 8, 256], BF16, "wuq")
        wukT_sb = P.sb([128, 8, 128], BF16, "wukT")
        wuv_sb = P.sb([128, 8, 128], BF16, "wuv")
        r_aw = Res("attw")
        A("pool", lambda e: e.dma_start(out=wuq_sb[:], in_=w_uq), writes=[r_aw], dma=True)
        A("pool", lambda e: e.dma_start(out=wukT_sb[:], in_=w_ukT), writes=[r_aw], dma=True)
        A("pool", lambda e: e.dma_start(out=wuv_sb[:], in_=w_uv), writes=[r_aw], dma=True)
        qn_ring = Ring(P, 2, [128, 512], BF16, "qn")
        qt_ring = Ring(P, 2, [128, 512], BF16, "qt")
        qpe_ring = Ring(P, 2, [128, 512], BF16, "qpe")
        for qb_, qr_ in zip(qpe_ring.bufs, qpe_ring.res):
            A("dve", lambda e: e.memset(qb_[64:128, :], 0.0), writes=[qr_])
        pT_ring = Ring(P, 6, [128, 512], BF16, "pT")
        dacc_ring = Ring(P, 2, [128, 512], F32, "dacc")
        rden_ring = Ring(P, 2, [128, 512], F32, "rden")
        oln_ring = Ring(P, 2, [128, 512], BF16, "oln")
        attc = P.sb([128, 8, 512], BF16, "attc")
        attr = Res("attc")
        qr1 = P.sb([64, 512], F32, "qr1")
        qr2 = P.sb([64, 512], F32, "qr2")
        r_qr = Res("qr")
        SB = [0, 1, 4, 5]
        LOOK = 3
        qcount = [0]

        def gen_q(ci, c0, W, s, hh):
            qn, qnr = qn_ring.next()
            qt, qtr = qt_ring.next()
            qpe, qper = qpe_ring.next()
            pq = 6 + (qcount[0] % 2)
            qcount[0] += 1

            def s1():
                for k2 in range(2):
                    A("pe", lambda e: e.matmul(ps[pq][:, :W], wuq_sb[:, k2, hh, 0:128], cqn[:, k2, c0:c0 + W], start=(k2 == 0), stop=(k2 == 1)),
                      reads=[r_aw, r_cqn[ci]], writes=[psr[pq]])
                A("act", lambda e: e.activation(out=qn[:, :W], in_=ps[pq][:, :W], func=AF.Copy), reads=[psr[pq]], writes=[qnr])

            def s2():
                A("pe", lambda e: e.matmul(ps[pq][:, :W], wukT_sb[:, hh, :], qn[:, :W], start=True, stop=True), reads=[r_aw, qnr], writes=[psr[pq]])
                A("act", lambda e: e.activation(out=qt[:, :W], in_=ps[pq][:, :W], func=AF.Identity, scale=ATTN_SCALE), reads=[psr[pq]], writes=[qtr])

            def s3():
                for k2 in range(2):
                    A("pe", lambda e: e.matmul(ps[pq][0:64, :W], wuq_sb[:, k2, hh, 128:192], cqn[:, k2, c0:c0 + W], start=(k2 == 0), stop=(k2 == 1)),
                      reads=[r_aw, r_cqn[ci]], writes=[psr[pq]])
                if s == 0:
                    A("dve", lambda e: e.tensor_tensor(out=qr1[:, :W], in0=ps[pq][0:64, :W], in1=rope_sb[:, 0, c0:c0 + W], op=ALU.mult),
                      reads=[psr[pq], r_const], writes=[r_qr])
                else:
                    A("act", lambda e: e.activation(out=qpe[0:64, :W], in_=ps[pq][0:64, :W], func=AF.Identity, scale=ATTN_SCALE), reads=[psr[pq]], writes=[qper])

            def s4():
                if s == 0:
                    for k2 in range(2):
                        A("pe", lambda e: e.matmul(ps[pq][0:64, :W], wuq_sb[:, k2, hh, 192:256], cqn[:, k2, c0:c0 + W], start=(k2 == 0), stop=(k2 == 1)),
                          reads=[r_aw, r_cqn[ci]], writes=[psr[pq]])
                    A("dve", lambda e: e.tensor_tensor(out=qr2[:, :W], in0=ps[pq][0:64, :W], in1=rope_sb[:, 1, c0:c0 + W], op=ALU.mult),
                      reads=[psr[pq], r_const, r_qr], writes=[r_qr])
                    A("dve", lambda e: e.tensor_tensor(out=qr1[:, :W], in0=qr1[:, :W], in1=qr2[:, :W], op=ALU.add), reads=[r_qr], writes=[r_qr])
                    A("act", lambda e: e.activation(out=qpe[0:64, :W], in_=qr1[:, :W], func=AF.Identity, scale=ATTN_SCALE), reads=[r_qr], writes=[qper])
            return (qt, qtr, qpe, qper), [(6, s1), (9, s2), (12, s3), (15, s4)]

        def make_epilogue(c0, W, hh, po, dacc, daccr):
            rden, rdr = rden_ring.next()
            oln, olr = oln_ring.next()
            st = {}

            def e1():
                pq = 6 + (qcount[0] % 2)
                qcount[0] += 1
                st["pq"] = pq
                A("pe", lambda e: e.matmul(ps[pq][:, :W], ones32[:], dacc[:, :W], start=True, stop=True), reads=[r_const, daccr], writes=[psr[pq]])
                A("dve", lambda e: e.reciprocal(out=rden[:, :W], in_=ps[pq][:, :W]), reads=[psr[pq]], writes=[rdr])
                A("dve", lambda e: e.tensor_tensor(out=oln[:, :W], in0=ps[po][:, :W], in1=rden[:, :W], op=ALU.mult), reads=[psr[po], rdr], writes=[olr])

            def e2():
                pq = st["pq"]
                A("pe", lambda e: e.matmul(ps[pq][:, :W], wuv_sb[:, hh, :], oln[:, :W], start=True, stop=True), reads=[r_aw, olr], writes=[psr[pq]])
                A("act", lambda e: e.activation(out=attc[:, hh, :W], in_=ps[pq][:, :W], func=AF.Copy), reads=[psr[pq]], writes=[attr])
                if hh == 7:
                    A("sp", lambda e: e.dma_start(out=att_scr[:, :, c0:c0 + W], in_=attc[:, :, :W]), reads=[attr], writes=[r_attscr], dma=True)
            return [(1, e1), (4, e2)]

        work = [(ci, c0, W, s, hh) for ci, (c0, W, s) in qchunks for hh in range(8)]
        qnext, qsteps = gen_q(*work[0])
        for _, fn_ in qsteps:
            fn_()
        pending = []
        sbi = 0
        for wi_, (ci, c0, W, s, hh) in enumerate(work):
            qt, qtr, qpe, qper = qnext
            kts = list(range(66)) if s == 0 else [0, 1]
            nk = len(kts)
            po = 2 + (wi_ % 2)
            if wi_ + 1 < len(work):
                qnext, qsteps = gen_q(*work[wi_ + 1])
                pending += qsteps

            def emit_qk(ki):
                kt = kts[ki]
                psb = SB[(sbi + ki) % 4]
                A("pe", lambda e: e.matmul(ps[psb][:, :W], ckvT[:, kt * 128:(kt + 1) * 128], qt[:, :W], start=True, stop=False),
                  reads=[r_kv, qtr], writes=[psr[psb]])
                A("pe", lambda e: e.matmul(ps[psb][:, :W], kpeT[:, kt * 128:(kt + 1) * 128], qpe[:, :W], start=False, stop=True),
                  reads=[r_kv, qper], writes=[psr[psb]])
            for ki in range(min(LOOK, nk)):
                emit_qk(ki)
            dacc, daccr = dacc_ring.next()
            for ki, kt in enumerate(kts):
                psb = SB[(sbi + ki) % 4]
                pT, pTr = pT_ring.next()
                A("act", lambda e: e.activation(out=pT[:, :W], in_=ps[psb][:, :W], func=AF.Exp), reads=[psr[psb]], writes=[pTr])
                if ki + LOOK < nk:
                    emit_qk(ki + LOOK)
                A("pe", lambda e: e.matmul(ps[po][:, :W], ckvtok[:, kt, :], pT[:, :W], start=(ki == 0), stop=(ki == nk - 1)),
                  reads=[r_kv, pTr], writes=[psr[po]])
                if ki == 0:
                    A("dve", lambda e: e.tensor_copy(out=dacc[:, :W], in_=pT[:, :W]), reads=[pTr], writes=[daccr])
                else:
                    A("dve", lambda e: e.tensor_tensor(out=dacc[:, :W], in0=dacc[:, :W], in1=pT[:, :W], op=ALU.add), reads=[pTr, daccr], writes=[daccr])
                due = [p_ for p_ in pending if p_[0] <= ki]
                pending = [p_ for p_ in pending if p_[0] > ki]
                for _, fn_ in due:
                    fn_()
            for _, fn_ in pending:
                fn_()
            pending = make_epilogue(c0, W, hh, po, dacc, daccr)
            sbi = (sbi + nk) % 4
        for _, fn_ in pending:
            fn_()
        P.barrier()
        P.sb_off = M0
        P.top = 228992

        alloc_norm()
        h1c = P.sb([128, 8, 512], BF16, "h1c")
        r_h1c = Res("h1c")
        attin = Ring(P, 2, [128, 8, 512], BF16, "attin")
        lruin = Ring(P, 2, [128, 8, 512], BF16, "lruin")
        mring = Ring(P, 2, [128, 8, 512], BF16, "mchunk")
        wm_ring = Ring(P, 12, [128, 8, 128], BF16, "wm")
        sg_ring = Ring(P, 4, [128, 512], F32, "sg")

        def load_wm(src, j):
            wt, wr = wm_ring.next()
            A("pool", lambda e: e.dma_start(out=wt[:], in_=src[j]), writes=[wr], dma=True)
            return wt, wr

        for ci, (c0, W, s) in qchunks:
            ai, air = attin.next()
            li, lir = lruin.next()
            A("sp", lambda e: e.dma_start(out=ai[:, :, :W], in_=att_scr[:, :, c0:c0 + W]), reads=[r_attscr], writes=[air], dma=True)
            A("sp", lambda e: e.dma_start(out=li[:, :, :W], in_=lru_scr[:, :, c0:c0 + W]), reads=[r_lruscr], writes=[lir], dma=True)
            norm_mod(ci, c0, W, s, 0, G_SH1, [(h1c, r_h1c, 0)])
            mc, mcr = mring.next()
            for j in range(8):
                woa_t, woa_r = load_wm(w_oa, j)
                wga_t, wga_r = load_wm(w_gatt, j)
                wol_t, wol_r = load_wm(w_ol, j)
                wgl_t, wgl_r = load_wm(w_glru, j)
                for k in range(8):
                    A("pe", lambda e: e.matmul(ps[0][:, :W], woa_t[:, k, :], ai[:, k, :W], start=(k == 0), stop=(k == 7)), reads=[woa_r, air], writes=[psr[0]])
                for k in range(8):
                    A("pe", lambda e: e.matmul(ps[1][:, :W], wga_t[:, k, :], h1c[:, k, :W], start=(k == 0), stop=(k == 7)), reads=[wga_r, r_h1c], writes=[psr[1]])
                for k in range(8):
                    A("pe", lambda e: e.matmul(ps[2][:, :W], wol_t[:, k, :], li[:, k, :W], start=(k == 0), stop=(k == 7)), reads=[wol_r, lir], writes=[psr[2]])
                for k in range(8):
                    A("pe", lambda e: e.matmul(ps[3][:, :W], wgl_t[:, k, :], h1c[:, k, :W], start=(k == 0), stop=(k == 7)), reads=[wgl_r, r_h1c], writes=[psr[3]])
                sa, sar = sg_ring.next()
                sl, slr = sg_ring.next()
                A("act", lambda e: e.activation(out=sa[:, :W], in_=ps[1][:, :W], func=AF.Sigmoid, bias=vecs_sb[:, j:j + 1]), reads=[psr[1], r_const], writes=[sar])
                A("act", lambda e: e.activation(out=sl[:, :W], in_=ps[3][:, :W], func=AF.Sigmoid, bias=vecs_sb[:, 8 + j:9 + j]), reads=[psr[3], r_const], writes=[slr])
                A("dve", lambda e: e.tensor_tensor(out=sa[:, :W], in0=sa[:, :W], in1=ps[0][:, :W], op=ALU.mult), reads=[sar, psr[0]], writes=[sar])
                A("dve", lambda e: e.tensor_tensor(out=sl[:, :W], in0=sl[:, :W], in1=ps[2][:, :W], op=ALU.mult), reads=[slr, psr[2]], writes=[slr])
                A("dve", lambda e: e.tensor_tensor(out=mc[:, j, :W], in0=sa[:, :W], in1=sl[:, :W], op=ALU.add), reads=[sar, slr], writes=[mcr])
            for i in range(8):
                wo_t, wo_r = load_wm(w_out, i)
                pb = 4 + (i % 2)
                for j in range(8):
                    A("pe", lambda e: e.matmul(ps[pb][:, :W], wo_t[:, j, :], mc[:, j, :W], start=(j == 0), stop=(j == 7)), reads=[wo_r, mcr], writes=[psr[pb]])
                A("dve", lambda e: e.scalar_tensor_tensor(out=xlat[:, i, c0:c0 + W], in0=ps[pb][:, :W], scalar=mod[:, G_G1 * 8 + i, s:s + 1],
                                                          in1=xlat[:, i, c0:c0 + W], op0=ALU.mult, op1=ALU.add),
                  reads=[psr[pb], r_mod, xlat_r[i][ci]], writes=[xlat_r[i][ci]])
        P.barrier()
        P.sb_off = M0

        h2 = P.sb([128, 8, NT], BF16, "h2")
        h2_r = [Res(f"h2_{c}") for c in range(5)]
        gt_sb = P.sb([NE, NT], F32, "gt_sb")
        r_gt = Res("gt")
        r_gtscr = Res("gtscr")
        wr_sb = P.sb([128, 8, NE], F32, "wr")
        br_sb = P.sb([128, NE], F32, "br")
        beg_sb = P.sb([128, NE, 8], F32, "beg")
        beu_sb = P.sb([128, NE, 8], F32, "beu")
        bed_sb = P.sb([NE, D], F32, "bed")
        r_mw = Res("moew")
        A("sp", lambda e: e.dma_start(out=wr_sb[:], in_=w_router), writes=[r_mw], dma=True)
        A("sp", lambda e: e.dma_start(out=br_sb[:], in_=b_router), writes=[r_mw], dma=True)
        A("sp", lambda e: e.dma_start(out=beg_sb[:], in_=beg), writes=[r_mw], dma=True)
        A("sp", lambda e: e.dma_start(out=beu_sb[:], in_=beu), writes=[r_mw], dma=True)
        A("sp", lambda e: e.dma_start(out=bed_sb[:], in_=bed), writes=[r_mw], dma=True)
        A("dve", lambda e: e.tensor_scalar(out=beu_sb[:], in0=beu_sb[:], scalar1=1.0, scalar2=None, op0=ALU.add), reads=[r_mw], writes=[r_mw])
        M2 = P.sb_off
        alloc_norm()
        h32 = P.sb([128, 8, 512], F32, "h32")
        r_h32 = Res("h32")
        lg = P.sb([128, NE], F32, "lg")
        m8 = P.sb([128, 8], F32, "m8")
        ex = P.sb([128, NE], F32, "ex")
        msk = P.sb([128, NE], F32, "msk")
        ssum = P.sb([128, 2], F32, "ssum")
        r_rt = Res("router")
        for ci, (c0, W, s) in qchunks:
            norm_mod(ci, c0, W, s, 1, G_SH2, [(h2, h2_r[ci], c0), (h32, r_h32, 0)])
            for t in range(W // 128):
                for k in range(8):
                    A("pe", lambda e: e.matmul(ps[6][:, 0:NE], h32[:, k, t * 128:(t + 1) * 128], wr_sb[:, k, :], start=(k == 0), stop=(k == 7)),
                      reads=[r_h32, r_mw], writes=[psr[6]])
                A("dve", lambda e: e.tensor_tensor(out=lg[:], in0=ps[6][:, 0:NE], in1=br_sb[:], op=ALU.add), reads=[psr[6], r_mw, r_rt], writes=[r_rt])
                A("dve", lambda e: e.max(out=m8[:], in_=lg[:]), reads=[r_rt], writes=[r_rt])
                A("dve", lambda e: e.tensor_scalar(out=msk[:], in0=lg[:], scalar1=m8[:, 3:4], scalar2=None, op0=ALU.is_ge), reads=[r_rt], writes=[r_rt])
                A("dve", lambda e: e.tensor_scalar(out=ssum[:, 0:1], in0=m8[:, 0:1], scalar1=-1.0, scalar2=None, op0=ALU.mult), reads=[r_rt], writes=[r_rt])
                A("act", lambda e: e.activation(out=ex[:], in_=lg[:], func=AF.Exp, bias=ssum[:, 0:1]), reads=[r_rt], writes=[r_rt])
                A("dve", lambda e: e.tensor_tensor(out=ex[:], in0=ex[:], in1=msk[:], op=ALU.mult), reads=[r_rt], writes=[r_rt])
                A("dve", lambda e: e.tensor_reduce(out=ssum[:, 1:2], in_=ex[:], axis=mybir.AxisListType.X, op=ALU.add), reads=[r_rt], writes=[r_rt])
                A("dve", lambda e: e.reciprocal(out=ssum[:, 1:2], in_=ssum[:, 1:2]), reads=[r_rt], writes=[r_rt])
                A("dve", lambda e: e.tensor_scalar(out=ex[:], in0=ex[:], scalar1=ssum[:, 1:2], scalar2=None, op0=ALU.mult), reads=[r_rt], writes=[r_rt])
                A("pe", lambda e: e.transpose(out=ps[5][0:NE, 0:128], in_=ex[:], identity=ident_sb[:]), reads=[r_rt, r_const], writes=[psr[5]])
                A("act", lambda e: e.activation(out=gt_sb[:, c0 + t * 128:c0 + (t + 1) * 128], in_=ps[5][0:NE, 0:128], func=AF.Copy), reads=[psr[5]], writes=[r_gt])
        A("sp", lambda e: e.dma_start(out=gt_scr, in_=gt_sb[:]), reads=[r_gt], writes=[r_gtscr], dma=True)
        P.barrier()
        P.sb_off = M2
        act_sb = P.sb([128, 8, NT], BF16, "act_sb")
        act_r = [Res(f"act{c}") for c in range(5)]
        wu_ring = Ring(P, 6, [128, 8, 128], BF16, "wunit")
        gbc = P.sb([128, NT], F32, "gbc")
        r_gbc = Res("gbc")
        tg = Ring(P, 2, [128, 512], F32, "tg")
        tsg = Ring(P, 2, [128, 512], F32, "tsg")
        tu = Ring(P, 2, [128, 512], F32, "tu")
        nW = sum(W for _, (c0, W, s) in qchunks)

        def load_unit(src, e_, f_):
            wt, wr = wu_ring.next()
            A("pool", lambda e: e.dma_start(out=wt[:], in_=src[e_, f_]), writes=[wr], dma=True)
            return wt, wr

        it = 0
        for ex_i in range(NE):
            A("sp", lambda e: e.dma_start(out=gbc[:, 0:nW], in_=AP(gt_scr.tensor, ex_i * NT, [[0, 128], [1, nW]])), reads=[r_gtscr], writes=[r_gbc], dma=True)
            for f in range(8):
                wg_t, wg_r = load_unit(weg, ex_i, f)
                wu_t, wu_r = load_unit(weu, ex_i, f)
                for ci, (c0, W, s) in qchunks:
                    pg = it % 2
                    pu = 2 + it % 2
                    it += 1
                    for k in range(8):
                        A("pe", lambda e: e.matmul(ps[pg][:, :W], wg_t[:, k, :], h2[:, k, c0:c0 + W], start=(k == 0), stop=(k == 7)), reads=[wg_r, h2_r[ci]], writes=[psr[pg]])
                    for k in range(8):
                        A("pe", lambda e: e.matmul(ps[pu][:, :W], wu_t[:, k, :], h2[:, k, c0:c0 + W], start=(k == 0), stop=(k == 7)), reads=[wu_r, h2_r[ci]], writes=[psr[pu]])
                    g_t, g_r = tg.next()
                    s_t, s_r = tsg.next()
                    u_t, u_r = tu.next()
                    A("dve", lambda e: e.tensor_scalar(out=g_t[:, :W], in0=ps[pg][:, :W], scalar1=beg_sb[:, ex_i, f:f + 1], scalar2=7.0, op0=ALU.add, op1=ALU.min),
                      reads=[psr[pg], r_mw], writes=[g_r])
                    A("act", lambda e: e.activation(out=s_t[:, :W], in_=g_t[:, :W], func=AF.Sigmoid, scale=1.702), reads=[g_r], writes=[s_r])
                    A("dve", lambda e: e.tensor_scalar(out=u_t[:, :W], in0=ps[pu][:, :W], scalar1=beu_sb[:, ex_i, f:f + 1], scalar2=8.0, op0=ALU.add, op1=ALU.min),
                      reads=[psr[pu], r_mw], writes=[u_r])
                    A("dve", lambda e: e.tensor_tensor(out=g_t[:, :W], in0=g_t[:, :W], in1=s_t[:, :W], op=ALU.mult), reads=[g_r, s_r], writes=[g_r])
                    A("dve", lambda e: e.scalar_tensor_tensor(out=u_t[:, :W], in0=u_t[:, :W], scalar=-6.0, in1=g_t[:, :W], op0=ALU.max, op1=ALU.mult),
                      reads=[u_r, g_r], writes=[u_r])
                    A("dve", lambda e: e.tensor_tensor(out=act_sb[:, f, c0:c0 + W], in0=u_t[:, :W], in1=gbc[:, c0:c0 + W], op=ALU.mult),
                      reads=[u_r, r_gbc], writes=[act_r[ci]])
            for d in range(8):
                wd_t, wd_r = load_unit(wed, ex_i, d)
                for ci, (c0, W, s) in qchunks:
                    py = 4 + it % 2
                    it += 1
                    for f in range(8):
                        A("pe", lambda e: e.matmul(ps[py][:, :W], wd_t[:, f, :], act_sb[:, f, c0:c0 + W], start=(f == 0), stop=(f == 7)), reads=[wd_r, act_r[ci]], writes=[psr[py]])
                    A("dve", lambda e: e.scalar_tensor_tensor(out=xlat[:, d, c0:c0 + W], in0=ps[py][:, :W], scalar=mod[:, G_G2 * 8 + d, s:s + 1],
                                                              in1=xlat[:, d, c0:c0 + W], op0=ALU.mult, op1=ALU.add),
                      reads=[psr[py], r_mod, xlat_r[d][ci]], writes=[xlat_r[d][ci]])
        for d in range(8):
            for ci, (c0, W, s) in qchunks:
                py = 4 + it % 2
                it += 1
                A("pe", lambda e: e.matmul(ps[py][:, :W], bed_sb[:, d * 128:(d + 1) * 128], gt_sb[:, c0:c0 + W], start=True, stop=True), reads=[r_mw, r_gt], writes=[psr[py]])
                A("dve", lambda e: e.scalar_tensor_tensor(out=xlat[:, d, c0:c0 + W], in0=ps[py][:, :W], scalar=mod[:, G_G2 * 8 + d, s:s + 1],
                                                          in1=xlat[:, d, c0:c0 + W], op0=ALU.mult, op1=ALU.add),
                  reads=[psr[py], r_mod, xlat_r[d][ci]], writes=[xlat_r[d][ci]])
        P.barrier()
        P.sb_off = M0

        P.barrier()

    layer(0)
    layer(1)
    P.sb_off = M0
    alloc_norm()
    oring = Ring(P, 2, [128, 8, 512], F32, "oring")
    for ci, (c0, W, s) in enumerate(chunks(False)):
        ob, obr = oring.next()
        norm_mod(ci, c0, W, 0, 2, None, [(ob, obr, 0)])
        finals.append(A("sp", lambda e: e.dma_start(out=yout[:, :, c0:c0 + W], in_=ob[:, :, :W]), reads=[obr], dma=True))
    P.emit(final_waits=finals)
    return nc, P


def _pk(a):
    return np.ascontiguousarray(a.reshape(8, 128, -1).transpose(1, 0, 2))


def _vec8(v):
    return np.ascontiguousarray(v.reshape(8, 128).T)


_ROPE_PERM = np.array([hf * 32 + (1 - pr) * 16 + j for hf in range(2) for pr in range(2) for j in range(16)])


def _rope_tables(q):
    t = np.arange(q * NL, (q + 1) * NL)
    row = (t // 64).astype(np.float32)
    col = (t % 64).astype(np.float32)
    inv = (np.float32(10000.0) ** (-np.arange(16, dtype=np.float32) / np.float32(16))).astype(np.float32)
    ang = np.concatenate([row[:, None] * inv, col[:, None] * inv], axis=-1).astype(np.float32)
    cos, sin = np.cos(ang).astype(np.float32), np.sin(ang).astype(np.float32)
    C = np.zeros((64, NL), np.float32)
    S = np.zeros((64, NL), np.float32)
    for hf in range(2):
        for pr in range(2):
            sl = slice(hf * 32 + pr * 16, hf * 32 + pr * 16 + 16)
            C[sl] = cos[:, hf * 16:(hf + 1) * 16].T
            S[sl] = sin[:, hf * 16:(hf + 1) * 16].T * (-1.0 if pr == 0 else 1.0)
    return np.ascontiguousarray(np.stack([C, S], axis=1))


def prep_layer(inp, l):
    f = np.float32
    w = {}
    w["w_ada"] = np.ascontiguousarray(inp["w_ada"][l].reshape(8, 128, 48, 128).transpose(2, 1, 0, 3))
    w["b_ada"] = np.ascontiguousarray(inp["b_ada"][l].reshape(48, 128).T)
    w["norms"] = np.ascontiguousarray(np.stack([_vec8(inp["norm_mix"][l]), _vec8(inp["norm_ffn"][l]), _vec8(inp["final_norm"])], axis=1))
    win = inp["w_in"][l]
    win_ext = np.concatenate([win, win[:, C_KPE:C_KPE + 64][:, _ROPE_PERM]], axis=1)
    w["w_in"] = _pk(win_ext)
    bbg = inp["b_branch_gate"][l].reshape(16, 128).T
    qn = inp["q_norm"][l].reshape(2, 128).T
    kvn = inp["kv_norm"][l].reshape(1, 128).T
    w["vecs"] = np.ascontiguousarray(np.concatenate([bbg, qn, kvn], axis=1)).astype(f)
    cw = inp["conv_w"][l]
    w["convp"] = np.ascontiguousarray(np.stack([_vec8(cw[0]), _vec8(cw[1]), _vec8(cw[2]), _vec8(cw[3]), _vec8(inp["conv_b"][l])], axis=1))
    w["lrup"] = np.ascontiguousarray(np.stack([np.stack([_vec8(a[d]) for d in range(2)], axis=1)
                                               for a in (inp["lru_b_a"][l], inp["lru_b_x"][l], inp["lru_lambda"][l])], axis=1))
    lw = np.zeros((8, 128, 2, 2, 128), f)
    for gi, g in enumerate((inp["lru_w_a"][l], inp["lru_w_x"][l])):
        for d in range(2):
            for c in range(8):
                lw[c, 0:64, gi, d, 0:64] = g[d, 2 * c]
                lw[c, 64:128, gi, d, 64:128] = g[d, 2 * c + 1]
    w["lru_w"] = lw
    wuq = inp["w_uq"][l].reshape(256, 8, 192)
    wuq_ext = np.concatenate([wuq, wuq[:, :, 128:192][:, :, _ROPE_PERM]], axis=2)
    w["w_uq"] = np.ascontiguousarray(wuq_ext.reshape(2, 128, 8, 256).transpose(1, 0, 2, 3))
    wukv = inp["w_ukv"][l].reshape(128, 8, 256)
    w["w_ukT"] = np.ascontiguousarray(wukv[:, :, 0:128].transpose(2, 1, 0))
    w["w_uv"] = np.ascontiguousarray(wukv[:, :, 128:256])

    def units(m):
        return np.ascontiguousarray(m.reshape(8, 128, 8, 128).transpose(2, 1, 0, 3))
    w["w_oa"] = units(inp["w_o_attn"][l])
    w["w_ol"] = units(inp["w_o_lru"][l])
    w["w_gatt"] = units(win[:, C_GATT:C_GATT + 1024])
    w["w_glru"] = units(win[:, C_GLRU:C_GLRU + 1024])
    w["w_out"] = units(inp["w_out"][l])
    w["w_router"] = _pk(inp["w_router"][l])
    w["b_router"] = np.ascontiguousarray(np.broadcast_to(inp["b_router"][l][None, :], (128, NE))).astype(f)

    def eunits(m):
        return np.ascontiguousarray(m.reshape(NE, 8, 128, 8, 128).transpose(0, 3, 2, 1, 4))
    w["weg"] = eunits(inp["w_exp_gate"][l])
    w["weu"] = eunits(inp["w_exp_up"][l])
    w["wed"] = eunits(inp["w_exp_down"][l])
    w["beg"] = np.ascontiguousarray(inp["b_exp_gate"][l].reshape(NE, 8, 128).transpose(2, 0, 1))
    w["beu"] = np.ascontiguousarray(inp["b_exp_up"][l].reshape(NE, 8, 128).transpose(2, 0, 1))
    w["bed"] = np.ascontiguousarray(inp["b_exp_down"][l])
    return w


def core_common(inp, xcores, j):
    b, q = j // 4, j % 4
    m = {"xin": xcores[j]}
    halo = np.zeros((128, 8, 3), np.float32)
    if q > 0:
        halo[:, :, 0:2] = xcores[j - 1][:, :, NL - 2:NL]
    if q < 3:
        halo[:, :, 2] = xcores[j + 1][:, :, 0]
    m["xhalo"] = halo
    m["edge"] = np.ascontiguousarray(np.broadcast_to(np.array([[q > 0, q < 3]], np.float32), (128, 2)))
    m["cc"] = np.ascontiguousarray(np.stack([_vec8(inp["c"][b]), _vec8(inp["c_ctx"])], axis=2))
    m["ropeT"] = _rope_tables(q)
    return m


W_KEYS = ["w_ada", "b_ada", "norms", "w_in", "vecs", "convp", "lrup", "lru_w", "w_uq", "w_ukT", "w_uv", "w_oa", "w_ol",
          "w_gatt", "w_glru", "w_out", "w_router", "b_router", "weg", "weu", "wed", "beg", "beu", "bed"]
_PROG = []


def kernel(**inputs):
    inp = {k: np.asarray(v, dtype=np.float32) for k, v in inputs.items()}
    x, ctx = inp["x"], inp["ctx"]
    xcores = []
    for j in range(8):
        b, q = j // 4, j % 4
        rows = np.concatenate([x[b, q * NL:(q + 1) * NL], ctx[b]], axis=0)
        xcores.append(_pk(np.ascontiguousarray(rows.T)))
    shared = {"ident": np.eye(128, dtype=np.float32)}
    for l in range(2):
        w = prep_layer(inp, l)
        for k in W_KEYS:
            shared[f"{k}_{l}"] = w[k]
    maps = []
    for j in range(8):
        q = j % 4
        m = dict(shared)
        m.update(core_common(inp, xcores, j))
        cm = np.zeros((128, 4, 2), np.float32)
        hm = np.zeros((128, 4, 2), np.float32)
        for i in range(4):
            cm[:, i, 0] = 1.0 if i < q else 0.0
            cm[:, i, 1] = 1.0 if i > q else 0.0
            hm[:, i, 0] = 1.0 if i == q - 1 else 0.0
            hm[:, i, 1] = 1.0 if i == q + 1 else 0.0
        m["cmask"] = cm
        m["hmask"] = hm
        maps.append(m)
    if not _PROG:
        _PROG.append(build_fused()[0])
    res = run_bass_kernel_spmd(_PROG[0], maps, core_ids=list(range(8))).results
    out = np.zeros((2, 4 * NL, D), np.float32)
    for j in range(8):
        b, q = j // 4, j % 4
        out[b, q * NL:(q + 1) * NL] = res[j]["yout"].transpose(2, 1, 0).reshape(NL, D)
    return out
```
